# Optimizing a Trainium2 kernel written in Bass

```python
import jax, jax.numpy as jnp
from jax import lax
import numpy as np

D_MODEL = 1024
BATCH = 8
SEQ = 8192
DEPTH = 1

GRID_W = 64
CTX_LEN = 256
CHUNK = 128
RET_HEADS = 4
RET_DK = 128
RET_DV = 128
ML_HEADS = 4
ML_DH = 128
RET_W = RET_HEADS * RET_DK
RET_VW = RET_HEADS * RET_DV
ML_W = ML_HEADS * ML_DH
N_ML_GATES = 4 * ML_HEADS
FFN_HIDDEN = -((-8 * D_MODEL) // (3 * 256)) * 256
ROPE_BASE = 10000.0
EPS = 1e-6
IN_WIDTHS = (RET_W, RET_W, RET_VW, RET_VW, ML_W, ML_W, ML_W, ML_W, N_ML_GATES, D_MODEL, D_MODEL)
IN_COLS = sum(IN_WIDTHS)

kernel_name = "hybrid_retention_mlstm_prefix_dit_block"


def rms_norm(x, gain):
    xf = x.astype(jnp.float32)
    y = xf * lax.rsqrt(jnp.mean(xf * xf, axis=-1, keepdims=True) + EPS)
    return (y * gain.astype(jnp.float32)).astype(x.dtype)


def head_norm(t):
    tf = t.astype(jnp.float32)
    return tf * lax.rsqrt(jnp.mean(tf * tf, axis=-1, keepdims=True) + EPS)


def modulate(h, shift, scale):
    return h * (1 + scale) + shift


def to_heads(t, n_heads):
    b, n, _ = t.shape
    return t.reshape(b, n, n_heads, -1).transpose(0, 2, 1, 3)


def from_heads(t):
    b, h, n, d = t.shape
    return t.transpose(0, 2, 1, 3).reshape(b, n, h * d)


def rope_2d(n):
    n_rows = n // GRID_W
    rows = jnp.broadcast_to(jnp.arange(n_rows, dtype=jnp.float32)[:, None], (n_rows, GRID_W)).reshape(n)
    cols = jnp.broadcast_to(jnp.arange(GRID_W, dtype=jnp.float32)[None, :], (n_rows, GRID_W)).reshape(n)
    n_freq = RET_DK // 4
    inv = ROPE_BASE ** (-jnp.arange(n_freq, dtype=jnp.float32) / n_freq)
    ang = jnp.concatenate([rows[:, None] * inv, cols[:, None] * inv], axis=-1)
    return jnp.cos(ang), jnp.sin(ang)


def apply_rope(t, cos, sin):
    half = t.shape[-1] // 2
    t1, t2 = t[..., :half], t[..., half:]
    cos = cos.astype(t.dtype)
    sin = sin.astype(t.dtype)
    return jnp.concatenate([t1 * cos - t2 * sin, t2 * cos + t1 * sin], axis=-1)


def to_chunks(t):
    b, h, n = t.shape[:3]
    t = t.reshape(b, h, n // CHUNK, CHUNK, *t.shape[3:])
    return jnp.moveaxis(t, 2, 0)


def from_chunks(t):
    t = jnp.moveaxis(t, 0, 2)
    b, h, nc, l = t.shape[:4]
    return t.reshape(b, h, nc * l, *t.shape[4:])


def retention_scan(q, k, v, log_gamma, state):
    q, k, v = (t.astype(jnp.float32) for t in (q, k, v))
    lg = log_gamma.astype(jnp.float32)
    pos = jnp.arange(CHUNK, dtype=jnp.float32)
    rel = pos[:, None] - pos[None, :]
    intra = jnp.where(rel >= 0, jnp.exp(lg[:, None, None] * jnp.maximum(rel, 0.0)), 0.0)
    inter = jnp.exp(lg[:, None] * (pos + 1.0))
    to_end = jnp.exp(lg[:, None] * (CHUNK - 1.0 - pos))
    chunk_decay = jnp.exp(lg * CHUNK)

    def step(s, inp):
        qc, kc, vc = inp
        scores = jnp.einsum("bhld,bhsd->bhls", qc, kc) * intra
        out = jnp.einsum("bhls,bhsv->bhlv", scores, vc) + inter[..., None] * jnp.einsum("bhld,bhdv->bhlv", qc, s)
        s_new = chunk_decay[:, None, None] * s + jnp.einsum("bhsd,bhsv->bhdv", kc * to_end[..., None], vc)
        return s_new, out

    final, outs = lax.scan(step, state, (to_chunks(q), to_chunks(k), to_chunks(v)))
    return from_chunks(outs), final


def mlstm_scan(q, k, v, i_pre, log_f, state):
    q, k, v, i_pre, log_f = (t.astype(jnp.float32) for t in (q, k, v, i_pre, log_f))
    causal = jnp.tril(jnp.ones((CHUNK, CHUNK), dtype=bool))

    def step(carry, inp):
        c_mat, n_vec, m = carry
        qc, kc, vc, ic, fc = inp
        b_cum = jnp.cumsum(fc, axis=-1)
        log_inter = b_cum + m[..., None]
        log_intra = jnp.where(causal, b_cum[..., :, None] - b_cum[..., None, :] + ic[..., None, :], -jnp.inf)
        m_t = jnp.maximum(log_inter, jnp.max(log_intra, axis=-1))
        w_inter = jnp.exp(log_inter - m_t)
        w_intra = jnp.exp(log_intra - m_t[..., None])
        s = jnp.einsum("bhld,bhsd->bhls", qc, kc) * w_intra
        num = jnp.einsum("bhls,bhsv->bhlv", s, vc) + w_inter[..., None] * jnp.einsum("bhvd,bhld->bhlv", c_mat, qc)
        den = jnp.sum(s, axis=-1) + w_inter * jnp.einsum("bhd,bhld->bhl", n_vec, qc)
        h = num / jnp.maximum(jnp.abs(den), jnp.exp(-m_t))[..., None]
        b_last = b_cum[..., -1]
        log_src = b_last[..., None] - b_cum + ic
        m_new = jnp.maximum(b_last + m, jnp.max(log_src, axis=-1))
        w_old = jnp.exp(b_last + m - m_new)
        w_src = jnp.exp(log_src - m_new[..., None])
        c_new = w_old[..., None, None] * c_mat + jnp.einsum("bhs,bhsv,bhsd->bhvd", w_src, vc, kc)
        n_new = w_old[..., None] * n_vec + jnp.einsum("bhs,bhsd->bhd", w_src, kc)
        return (c_new, n_new, m_new), h

    final, outs = lax.scan(step, state, tuple(to_chunks(t) for t in (q, k, v, i_pre, log_f)))
    return from_chunks(outs), final


def zero_states(b):
    ret = jnp.zeros((b, RET_HEADS, RET_DK, RET_DV), jnp.float32)
    ml = (jnp.zeros((b, ML_HEADS, ML_DH, ML_DH), jnp.float32),
          jnp.zeros((b, ML_HEADS, ML_DH), jnp.float32),
          jnp.zeros((b, ML_HEADS), jnp.float32))
    return (ret, ml)


def mixer_inputs(h, w_in, gate_bias, rope):
    offsets = np.cumsum(IN_WIDTHS)[:-1].tolist()
    rq, rk, rv, rg, mq, mk, mv, mo, mg, bg_ret, bg_ml = (h @ w for w in jnp.split(w_in, offsets, axis=-1))
    rq = to_heads(rq, RET_HEADS)
    rk = to_heads(rk, RET_HEADS) * (RET_DK ** -0.5)
    if rope is not None:
        rq = apply_rope(rq, *rope)
        rk = apply_rope(rk, *rope)
    rv = to_heads(rv, RET_HEADS)
    mq = to_heads(mq, ML_HEADS)
    mk = to_heads(mk, ML_HEADS) * (ML_DH ** -0.5)
    mv = to_heads(mv, ML_HEADS)
    b, n, _ = mg.shape
    mg = (mg + gate_bias.reshape(-1)).reshape(b, n, 4, ML_HEADS).transpose(2, 0, 3, 1)
    i_f, lf_f, i_b, lf_b = mg[0], jax.nn.log_sigmoid(mg[1]), mg[2], jax.nn.log_sigmoid(mg[3])
    feats = (rq, rk, rv, mq, mk, mv, i_f, lf_f, i_b, lf_b)
    post = (rg, mo, bg_ret, bg_ml)
    return feats, post


def bidirectional_scans(feats, log_gamma, st_f, st_b):
    rq, rk, rv, mq, mk, mv, i_f, lf_f, i_b, lf_b = feats
    flip = lambda t: jnp.flip(t, axis=2)
    ret_f, r_state_f = retention_scan(rq, rk, rv, log_gamma[0], st_f[0])
    ml_f, m_state_f = mlstm_scan(mq, mk, mv, i_f, lf_f, st_f[1])
    ret_b, r_state_b = retention_scan(flip(rq), flip(rk), flip(rv), log_gamma[1], st_b[0])
    ml_b, m_state_b = mlstm_scan(flip(mq), flip(mk), flip(mv), flip(i_b), flip(lf_b), st_b[1])
    return ret_f + flip(ret_b), ml_f + flip(ml_b), (r_state_f, m_state_f), (r_state_b, m_state_b)


def mixer_output(ret, ml, rg, mo, bg_ret, bg_ml, w_ret_up, w_ml_up, w_out):
    dtype = rg.dtype
    y_ret = from_heads(head_norm(ret)).astype(dtype) * jax.nn.silu(rg)
    y_ml = from_heads(head_norm(jax.nn.sigmoid(to_heads(mo, ML_HEADS)) * ml)).astype(dtype)
    merged = jax.nn.sigmoid(bg_ret) * (y_ret @ w_ret_up) + jax.nn.sigmoid(bg_ml) * (y_ml @ w_ml_up)
    return merged @ w_out


def swiglu(h, w_ffn_in, w_ffn_out):
    gate, up = jnp.split(h @ w_ffn_in, 2, axis=-1)
    return (jax.nn.silu(gate) * up) @ w_ffn_out


def setup_inputs(seed: int = 0) -> dict:
    key = jax.random.key(seed)
    ks = jax.random.split(key, 20)
    f32 = jnp.float32

    def nrm(k, shape, scale):
        return jax.random.normal(k, shape, f32) * scale

    x = nrm(ks[0], (BATCH, SEQ, D_MODEL), 1.0)
    c = nrm(ks[1], (BATCH, D_MODEL), 1.0)
    ctx = nrm(ks[2], (BATCH, CTX_LEN, D_MODEL), 1.0)
    c_ctx = nrm(ks[3], (D_MODEL,), 1.0)
    w_ada = nrm(ks[4], (DEPTH, D_MODEL, 6 * D_MODEL), 0.5 * D_MODEL ** -0.5)
    b_ada = nrm(ks[5], (DEPTH, 6 * D_MODEL), 0.02)
    norm1_gain = 1.0 + nrm(ks[6], (DEPTH, D_MODEL), 0.02)
    norm2_gain = 1.0 + nrm(ks[7], (DEPTH, D_MODEL), 0.02)
    w_in = nrm(ks[8], (DEPTH, D_MODEL, IN_COLS), D_MODEL ** -0.5)
    i_bias = nrm(ks[9], (DEPTH, 2, ML_HEADS), 0.1)
    f_bias = jnp.linspace(3.0, 6.0, ML_HEADS, dtype=f32) + nrm(ks[10], (DEPTH, 2, ML_HEADS), 0.1)
    mlstm_gate_bias = jnp.stack([i_bias[:, 0], f_bias[:, 0], i_bias[:, 1], f_bias[:, 1]], axis=1)
    gamma = 1.0 - 2.0 ** (-5.0 - jnp.arange(RET_HEADS, dtype=f32))
    ret_decay_logit = jnp.log(gamma / (1.0 - gamma)) + nrm(ks[11], (DEPTH, 2, RET_HEADS), 0.1)
    w_ret_up = nrm(ks[12], (DEPTH, RET_VW, D_MODEL), RET_VW ** -0.5)
    w_ml_up = nrm(ks[13], (DEPTH, ML_W, D_MODEL), ML_W ** -0.5)
    w_out = nrm(ks[14], (DEPTH, D_MODEL, D_MODEL), D_MODEL ** -0.5)
    w_ffn_in = nrm(ks[15], (DEPTH, D_MODEL, 2 * FFN_HIDDEN), D_MODEL ** -0.5)
    w_ffn_out = nrm(ks[16], (DEPTH, FFN_HIDDEN, D_MODEL), FFN_HIDDEN ** -0.5)
    final_gain = 1.0 + nrm(ks[17], (D_MODEL,), 0.02)
    return {"x": x, "c": c, "ctx": ctx, "c_ctx": c_ctx, "w_ada": w_ada, "b_ada": b_ada,
            "norm1_gain": norm1_gain, "norm2_gain": norm2_gain, "w_in": w_in,
            "mlstm_gate_bias": mlstm_gate_bias, "ret_decay_logit": ret_decay_logit,
            "w_ret_up": w_ret_up, "w_ml_up": w_ml_up, "w_out": w_out,
            "w_ffn_in": w_ffn_in, "w_ffn_out": w_ffn_out, "final_gain": final_gain}


def reference(x, c, ctx, c_ctx, w_ada, b_ada, norm1_gain, norm2_gain, w_in, mlstm_gate_bias,
              ret_decay_logit, w_ret_up, w_ml_up, w_out, w_ffn_in, w_ffn_out, final_gain):
    b, n, _ = x.shape
    rope = rope_2d(n)
    log_gamma = jax.nn.log_sigmoid(ret_decay_logit.astype(jnp.float32))
    for layer in range(DEPTH):
        mod = jax.nn.silu(c) @ w_ada[layer] + b_ada[layer]
        sh1, sc1, g1, sh2, sc2, g2 = jnp.split(mod[:, None, :], 6, axis=-1)
        mod_c = jax.nn.silu(c_ctx) @ w_ada[layer] + b_ada[layer]
        csh1, csc1, cg1, csh2, csc2, cg2 = jnp.split(mod_c, 6)

        hc = modulate(rms_norm(ctx, norm1_gain[layer]), csh1, csc1)
        feats_c, post_c = mixer_inputs(hc, w_in[layer], mlstm_gate_bias[layer], None)
        zero = zero_states(b)
        ret_c, ml_c, st_f, st_b = bidirectional_scans(feats_c, log_gamma[layer], zero, zero)

        hx = modulate(rms_norm(x, norm1_gain[layer]), sh1, sc1)
        feats_x, post_x = mixer_inputs(hx, w_in[layer], mlstm_gate_bias[layer], rope)
        ret_x, ml_x, _, _ = bidirectional_scans(feats_x, log_gamma[layer], st_f, st_b)
        x = x + g1 * mixer_output(ret_x, ml_x, *post_x, w_ret_up[layer], w_ml_up[layer], w_out[layer])
        x = x + g2 * swiglu(modulate(rms_norm(x, norm2_gain[layer]), sh2, sc2), w_ffn_in[layer], w_ffn_out[layer])

        if layer + 1 < DEPTH:
            ctx = ctx + cg1 * mixer_output(ret_c, ml_c, *post_c, w_ret_up[layer], w_ml_up[layer], w_out[layer])
            ctx = ctx + cg2 * swiglu(modulate(rms_norm(ctx, norm2_gain[layer]), csh2, csc2),
                                     w_ffn_in[layer], w_ffn_out[layer])
    return rms_norm(x, final_gain)
```

```python
import numpy as np
from contextlib import ExitStack
import concourse.bass as bass
import concourse.mybir as mybir
from concourse.bass_utils import run_bass_kernel_spmd

F32 = mybir.dt.float32
BF16 = mybir.dt.bfloat16
AF = mybir.ActivationFunctionType
ALU = mybir.AluOpType

D = 1024
CH = 128
NCTX = 2
HID = 2816
EPS = 1e-6
KS = 128.0 ** -0.5

C_TRIF, C_TRIB, C_ONES, C_RELF, C_RELB, C_LP1, C_LM = 0, 128, 256, 384, 512, 640, 768
C_COLS, C_SEL, C_I2, C_ID = 896, 898, 1026, 1028
NCST = 1156


class Op:
    __slots__ = ("eng", "fn", "deps", "is_dma", "signal", "tok", "idx", "stream")


class Prog:
    ENGS = ("pe", "act", "dve", "pool", "sp")

    def __init__(self, nc, es):
        self.nc = nc
        self.es = es
        self.eng_sets = [{e: es.enter_context(nc.semaphore("sem%d_%s" % (k, e))) for e in self.ENGS}
                         for k in range(3)]
        self.seg = 0
        self.stream_sem = {}
        self.stream_cnt = {}
        self.seen = {e: {} for e in self.ENGS}
        self.final = {}
        self._reset()

    def _reset(self):
        self.ops = {e: [] for e in self.ENGS}
        self.lastw = {}
        self.readers = {}

    def add(self, eng, fn, reads=(), writes=(), stream=None):
        op = Op()
        op.eng, op.fn, op.is_dma, op.stream = eng, fn, stream is not None, stream
        op.signal = op.is_dma
        op.tok = None
        op.idx = len(self.ops[eng])
        deps = {}
        for r in reads:
            w = self.lastw.get(r)
            if w is not None:
                deps[id(w)] = (w, 0)
        for k in writes:
            w = self.lastw.get(k)
            if w is not None:
                deps[id(w)] = (w, 0)
            for rd in self.readers.get(k, ()):
                if id(rd) not in deps:
                    deps[id(rd)] = (rd, 1)
        keep = []
        for d, war in deps.values():
            if d.eng == eng and not d.is_dma and not op.is_dma:
                if eng == "pe" or war:
                    continue
                if op.idx - d.idx > 4:
                    continue
            d.signal = True
            keep.append(d)
        op.deps = keep
        for r in reads:
            self.readers.setdefault(r, []).append(op)
        for k in writes:
            self.lastw[k] = op
            self.readers[k] = []
        self.ops[eng].append(op)
        return op

    def emit(self):
        nc = self.nc
        cur = self.seg % 3
        nxt = (self.seg + 1) % 3
        self.seg += 1
        eng_sem = self.eng_sets[cur]
        eng_cnt = {e: 0 for e in self.ENGS}
        for e in self.ENGS:
            self.seen[e] = {k: v for k, v in self.seen[e].items() if not k.startswith("e")}
        self.final = {k: v for k, v in self.final.items() if not k.startswith("e")}
        for e in self.ENGS:
            if self.ops[e]:
                self.ops[e][-1].signal = True
        for e in self.ENGS:
            for op in self.ops[e]:
                if not op.signal:
                    continue
                if op.is_dma:
                    if op.stream not in self.stream_sem:
                        self.stream_sem[op.stream] = self.es.enter_context(
                            nc.semaphore("dma_" + op.stream))
                        self.stream_cnt[op.stream] = 0
                    self.stream_cnt[op.stream] += 16
                    op.tok = (self.stream_sem[op.stream], self.stream_cnt[op.stream], "s" + op.stream)
                    self.final["s" + op.stream] = op.tok
                else:
                    eng_cnt[e] += 1
                    op.tok = (eng_sem[e], eng_cnt[e], "e" + e)
                    self.final["e" + e] = op.tok
        clear_sems = self.eng_sets[nxt]
        finals = list(self.final.values())

        def run(ename, eng):
            seen = self.seen[ename]
            eng.sem_clear(clear_sems[ename])
            for op in self.ops[ename]:
                for d in op.deps:
                    sem, val, key = d.tok
                    if seen.get(key, 0) < val:
                        eng.wait_ge(sem, val)
                        seen[key] = val
                inst = op.fn(eng)
                if op.signal:
                    inst.then_inc(op.tok[0], 16 if op.is_dma else 1)
            for sem, val, key in finals:
                if seen.get(key, 0) < val:
                    eng.wait_ge(sem, val)
                    seen[key] = val

        with nc.Block() as block:
            @block.tensor
            def _(e):
                run("pe", e)

            @block.scalar
            def _(e):
                run("act", e)

            @block.vector
            def _(e):
                run("dve", e)

            @block.gpsimd
            def _(e):
                run("pool", e)

            @block.sync
            def _(e):
                run("sp", e)
        self._reset()


class Rot:
    uid = 0

    def __init__(self, es, alloc, name, n, shape, dtype):
        Rot.uid += 1
        self.tiles = [es.enter_context(alloc(f"r{Rot.uid}_{name}{i}", shape, dtype)) for i in range(n)]
        self.name, self.n, self.i = name, n, 0

    def next(self):
        k = self.i % self.n
        self.i += 1
        return self.tiles[k], (self.name, k)


class _Stop(Exception):
    pass


STOP = -1


def chk(k):
    if STOP == k:
        raise _Stop()


def sub(P, k):
    if STOP == 100 + k:
        P.emit()
        raise _Stop()


def build_nc(NCH, dbg=False):
    nc = bass.Bass("TRN2", target_bir_lowering=False)
    try:
        _build(nc, NCH)
    except _Stop:
        pass
    return nc


def _build(nc, NCH):
    NT = NCH * CH

    def din(name, shape, dt=F32):
        return nc.dram_tensor(name, list(shape), dt, kind="ExternalInput").ap()

    x_h = din("x", [NT, D])
    ctx_h = din("ctx", [NCTX * CH, D])
    cc_h = din("cc", [128, 16])
    wada_h = din("w_ada", [D, 6 * D])
    bada_h = din("b_ada", [1, 6 * D])
    gains_h = din("gains", [128, 16])
    fg_h = din("final_gain", [1, D])
    wkv_h = din("w_kv", [D, 2064])
    wq_h = din("w_q", [D, 1024])
    wpost_h = din("w_post", [D, 3072])
    gb_h = din("gate_bias", [1, 16])
    rdl_h = din("ret_decay", [1, 8])
    wrup_h = din("w_ret_up", [512, D])
    wmup_h = din("w_ml_up", [512, D])
    wout_h = din("w_out", [D, D])
    wffi_h = din("w_ffn_in", [D, 2 * HID])
    wffo_h = din("w_ffn_out", [HID, D])
    cst_h = din("cst", [128, NCST])
    rope_h = din("rope", [NCH, 128, 256])
    y_h = nc.dram_tensor("y", [NT, D], F32, kind="ExternalOutput").ap()
    sb_h = nc.dram_tensor("sb_scr", [NCH, 128, 8 * 130], BF16, kind="Internal").ap()
    raw_h = y_h
    x1_h = y_h

    with ExitStack() as es:
        P = Prog(nc, es)

        def sb(name, shape, dt=F32, scope=None):
            Rot.uid += 1
            return (scope or es).enter_context(nc.sbuf_tensor(f"t{Rot.uid}_{name}", list(shape), dt))

        cst = sb("cst", [128, NCST])
        ident = sb("ident", [128, 128], BF16)
        modT = sb("modT", [128, 48, 2])
        vec = sb("vec", [128, 6, 8])
        gains = sb("gains", [128, 16])
        bc = sb("bc", [128, 3, D])
        gbias = sb("gbias", [128, 16])
        rstd2 = sb("rstd2", [128, NCH])
        junk = sb("junk", [128, D], BF16)
        kvs = ExitStack()
        lg = sb("lg", [128, 8], scope=kvs)
        Mret = sb("Mret", [128, 4, 128], scope=kvs)
        cfb = sb("cfb", [128, 8, 128], BF16, scope=kvs)
        rconst = sb("rconst", [128, 16], scope=kvs)
        S_f = sb("S_f", [128, 8, 129], scope=kvs)
        S_b = sb("S_b", [128, 8, 129], scope=kvs)
        Sf_bf = sb("Sf_bf", [128, 8, 130], BF16, scope=kvs)
        Sb_bf = [sb("Sb_bf0", [128, 8, 130], BF16, scope=kvs), sb("Sb_bf1", [128, 8, 130], BF16, scope=kvs)]

        psA = Rot(es, nc.psum_tensor, "psA", 4, [128, 512], F32)
        psS_t = [es.enter_context(nc.psum_tensor(f"psS{i}", [128, 512], F32)) for i in range(2)]
        psT_t = [es.enter_context(nc.psum_tensor(f"psT{i}", [128, 512], BF16)) for i in range(2)]
        cnt = {"S": 0, "T": 0}

        def psS():
            k = cnt["S"] % 4
            cnt["S"] += 1
            return psS_t[k // 2][:, (k % 2) * 256:(k % 2) * 256 + 256], ("psS", k)

        def psT():
            k = cnt["T"] % 2
            cnt["T"] += 1
            return psT_t[k][:, 0:512], ("psT", k)

        def dma(out, in_, reads, writes, stream, q="sp", slow=False):
            q = "sp"
            if slow:
                P.add(q, lambda e: e.dma_start(out=out, in_=in_, allow_slow_non_contiguous=True),
                      reads, writes, stream=stream)
            else:
                P.add(q, lambda e: e.dma_start(out=out, in_=in_), reads, writes, stream=stream)

        def act(out, in_, func, reads, writes, bias=0.0, scale=1.0, accum=None):
            if accum is None:
                P.add("act", lambda e: e.activation(out=out, in_=in_, func=func, bias=bias, scale=scale),
                      reads, writes)
            else:
                P.add("act", lambda e: e.activation(out=out, in_=in_, func=func, bias=bias, scale=scale,
                                                    accum_out=accum), reads, writes)

        def tt(eng, out, in0, in1, op, reads, writes):
            if eng == "pool":
                eng = "dve"
            P.add(eng, lambda e: e.tensor_tensor(out=out, in0=in0, in1=in1, op=op), reads, writes)

        def ts(eng, out, in0, s1, s2, op0, op1, reads, writes):
            if op1 is None and op0 == ALU.mult:
                P.add(eng, lambda e: e.tensor_scalar_mul(out=out, in0=in0, scalar1=s1), reads, writes)
            elif op1 is None and op0 == ALU.max:
                P.add(eng, lambda e: e.tensor_scalar_max(out=out, in0=in0, scalar1=s1), reads, writes)
            else:
                P.add(eng, lambda e: e.tensor_scalar(out=out, in0=in0, scalar1=s1, scalar2=s2,
                                                     op0=op0, op1=op1), reads, writes)

        def stt(eng, out, in0, scalar, in1, op0, op1, reads, writes):
            P.add(eng, lambda e: e.scalar_tensor_tensor(out=out, in0=in0, scalar=scalar, in1=in1,
                                                        op0=op0, op1=op1), reads, writes)

        def cp(eng, out, in_, reads, writes):
            if eng == "pool":
                eng = "dve"
            if eng == "act":
                act(out, in_, AF.Copy, reads, writes)
            else:
                P.add(eng, lambda e: e.tensor_copy(out=out, in_=in_), reads, writes)

        def mm(out, lhsT, rhs, start, stop, reads, writes):
            P.add("pe", lambda e: e.matmul(out, lhsT=lhsT, rhs=rhs, start=start, stop=stop), reads, writes)

        def tr(out, in_, reads, writes):
            P.add("pe", lambda e: e.transpose(out, in_, ident[:]), list(reads) + ["ident"], writes)

        def rsqrt_act(out, in_, reads, writes, tmp, tmpk, scale=1.0):
            act(tmp, in_, AF.Ln, list(reads), [tmpk], bias=EPS, scale=scale)
            act(out, tmp, AF.Exp, [tmpk], writes, scale=-0.5)

        with ExitStack() as ph:
            dma(cst[:], cst_h[:, :], [], ["cst"], "c0")
            dma(gains[:], gains_h[:, :], [], ["gains"], "c1")
            dma(gbias[:], gb_h[0:1, :].partition_broadcast(128), [], ["gbias"], "c2")
            rdl = sb("rdl", [128, 8], scope=ph)
            dma(rdl[:], rdl_h[0:1, :].partition_broadcast(128), [], ["rdl"], "c3")
            dma(bc[:, 2, :], fg_h[0:1, :].partition_broadcast(128), [], ["fgbc"], "c4")
            ccol = sb("ccol", [128, 16], scope=ph)
            dma(ccol[:], cc_h[:, :], [], ["ccol"], "c5")
            modrow = sb("modrow", [2, 6 * D], scope=ph)
            bada2 = sb("bada2", [2, 6 * D], scope=ph)
            dma(bada2[:], bada_h[0:1, :].partition_broadcast(2), [], ["bada2"], "c6")

            cp("dve", ident[:], cst[:, C_ID:C_ID + 128], ["cst"], ["ident"])
            t1 = sb("su_t1", [128, 16], scope=ph)
            t2 = sb("su_t2", [128, 16], scope=ph)
            sc = sb("su_sc", [128, 16], scope=ph)
            act(t1[:], ccol[:], AF.Exp, ["ccol"], ["su1"], scale=-1.0)
            act(t2[:], t1[:], AF.Ln, ["su1"], ["su2"], bias=1.0)
            act(t1[:], t2[:], AF.Exp, ["su2"], ["su1"], scale=-1.0)
            tt("dve", sc[:], ccol[:], t1[:], ALU.mult, ["ccol", "su1"], ["sc"])
            scv = sc[:].rearrange("p (j t) -> p j t", t=2)
            wst = Rot(ph, nc.sbuf_tensor, "wadast", 2, [128, 8, 512], F32)
            wada_v = wada_h.rearrange("(j p) c -> p j c", p=128)
            for cb in range(12):
                wt, wk = wst.next()
                dma(wt[:], wada_v[:, :, cb * 512:(cb + 1) * 512], [], [wk], "wa%d" % (cb % 2),
                    q=("sp" if cb % 2 == 0 else "pool"))
                pa, pk = psA.next()
                for j in range(8):
                    mm(pa[0:2, :], scv[:, j, :], wt[:, j, :], j == 0, j == 7, ["sc", wk], [pk])
                tt("dve", modrow[:, cb * 512:(cb + 1) * 512], pa[0:2, :], bada2[:, cb * 512:(cb + 1) * 512],
                   ALU.add, [pk, "bada2"], ["modrow"])
            pa, pk = psA.next()
            for blk in range(48):
                mm(pa[:, blk * 2:blk * 2 + 2], modrow[0:2, blk * 128:(blk + 1) * 128],
                   cst[0:2, C_I2:C_I2 + 2], True, True, ["modrow", "cst"], [pk])
            cp("dve", modT[:].rearrange("p a b -> p (a b)"), pa[:, 0:96], [pk], ["modT"])
            stt("dve", vec[:, 0, :], modT[:, 8:16, 0], 1.0, gains[:, 0:8], ALU.add, ALU.mult,
                ["modT", "gains"], ["vec0"])
            cp("dve", vec[:, 1, :], modT[:, 0:8, 0], ["modT"], ["vec1"])
            stt("dve", vec[:, 2, :], modT[:, 8:16, 1], 1.0, gains[:, 0:8], ALU.add, ALU.mult,
                ["modT", "gains"], ["vec2"])
            cp("dve", vec[:, 3, :], modT[:, 0:8, 1], ["modT"], ["vec3"])
            stt("dve", vec[:, 4, :], modT[:, 32:40, 0], 1.0, gains[:, 8:16], ALU.add, ALU.mult,
                ["modT", "gains"], ["vec4"])
            cp("dve", vec[:, 5, :], modT[:, 24:32, 0], ["modT"], ["vec5"])
            for gi, c0 in ((0, 2 * D), (1, 5 * D)):
                for cb in range(2):
                    pa, pk = psA.next()
                    mm(pa[:, :], cst[0:2, C_SEL:C_SEL + 128], modrow[0:2, c0 + cb * 512:c0 + (cb + 1) * 512],
                       True, True, ["cst", "modrow"], [pk])
                    cp("dve", bc[:, gi, cb * 512:(cb + 1) * 512], pa[:, :], [pk], ["bc%d" % gi])
            r1 = sb("r1", [128, 8], scope=ph)
            act(r1[:], rdl[:], AF.Exp, ["rdl"], ["r1"], scale=-1.0)
            act(lg[:], r1[:], AF.Ln, ["r1"], ["lgp"], bias=1.0)
            ts("dve", lg[:], lg[:], -1.0, None, ALU.mult, None, ["lgp"], ["lg"])
            mtmp = sb("mtmp", [128, 2, 128], scope=ph)
            for h in range(4):
                act(mtmp[:, 0, :], cst[:, C_RELF:C_RELF + 128], AF.Exp, ["cst", "lg"], ["mt0"], scale=lg[:, h:h + 1])
                act(mtmp[:, 1, :], cst[:, C_RELB:C_RELB + 128], AF.Exp, ["cst", "lg"], ["mt1"],
                    scale=lg[:, 4 + h:5 + h])
                tt("dve", mtmp[:, 0, :], mtmp[:, 0, :], cst[:, C_TRIF:C_TRIF + 128], ALU.mult, ["mt0", "cst"], ["mt0"])
                tt("dve", mtmp[:, 1, :], mtmp[:, 1, :], cst[:, C_TRIB:C_TRIB + 128], ALU.mult, ["mt1", "cst"], ["mt1"])
                tt("dve", Mret[:, h, :], mtmp[:, 0, :], mtmp[:, 1, :], ALU.add, ["mt0", "mt1"], ["Mret"])
                act(cfb[:, h, :], cst[:, C_LP1:C_LP1 + 128], AF.Exp, ["cst", "lg"], ["cfb"], scale=lg[:, h:h + 1])
                act(cfb[:, 4 + h, :], cst[:, C_LM:C_LM + 128], AF.Exp, ["cst", "lg"], ["cfb"],
                    scale=lg[:, 4 + h:5 + h])
                act(rconst[:, h:h + 1], cst[:, C_COLS:C_COLS + 1], AF.Exp, ["cst", "lg"], ["rconst"],
                    scale=lg[:, h:h + 1])
                act(rconst[:, 4 + h:5 + h], cst[:, C_COLS + 1:C_COLS + 2], AF.Exp, ["cst", "lg"], ["rconst"],
                    scale=lg[:, 4 + h:5 + h])
            act(rconst[:, 8:16], lg[:], AF.Exp, ["lg"], ["rconst"], scale=128.0)
            for t, k in ((S_f, "S_f"), (S_b, "S_b")):
                P.add("pool", lambda e, t=t: e.memset(t[:], 0.0), [], [k])
            P.add("pool", lambda e: e.memset(Sf_bf[:], 0.0), [], ["Sf_bf"])
            P.add("pool", lambda e: e.memset(Sb_bf[0][:], 0.0), [], ["Sb_bf0"])
            P.emit(); chk(0)

        lc_n = [0]

        def load_cast(dst, src_h, J, C, name, colscale=None, scope=None, st=None):
            if st is None:
                st = Rot(scope, nc.sbuf_tensor, "wst_" + name, 2, [128, 3072], F32)
            engs = ("dve", "act")
            for j in range(J):
                for c0 in range(0, C, 3072):
                    c1 = min(C, c0 + 3072)
                    t, k = st.next()
                    n = k[1]
                    dma(t[:, 0:c1 - c0], src_h[j * 128:(j + 1) * 128, c0:c1], [], [k],
                        "wst%d" % n, q="sp")
                    if colscale is not None:
                        tt("dve", dst[:, j, c0:c1], t[:, 0:c1 - c0], colscale[:, c0:c1], ALU.mult,
                           [k, "bc0", "bc1"], [name])
                    else:
                        cp(engs[lc_n[0] % 2], dst[:, j, c0:c1], t[:, 0:c1 - c0], [k], [name])
                    lc_n[0] += 1

        def front_end(src_rows, gm_i, sh_i, R, xkey_stream):
            xt, xk = R["x"].next()
            dma(xt[:], src_rows, [], [xk], xkey_stream + str(xk[1]))
            sst, ssk = R["ss"].next()
            sub(P, 10)
            act(junk[:], xt[:], AF.Square, [xk], ["junk", ssk], scale=1.0 / 32.0, accum=sst[:, 0:1])
            sub(P, 11)
            rsqrt_act(sst[:, 2:3], sst[:, 0:1], [ssk], [ssk], sst[:, 1:2], ssk)
            sub(P, 12)
            xn, xnk = R["xn"].next()
            ts("dve", xn[:], xt[:], sst[:, 2:3], None, ALU.mult, None, [xk, ssk], [xnk])
            sub(P, 13)
            hT, hk = R["hT"].next()
            for half in range(2):
                pt, ptk = psT()
                for jj in range(4):
                    j = half * 4 + jj
                    tr(pt[:, jj * 128:(jj + 1) * 128], xn[:, j * 128:(j + 1) * 128], [xnk], [ptk])
                sub(P, 14)
                for jj in range(4):
                    j = half * 4 + jj
                    ts("dve", hT[:, j, :], pt[:, jj * 128:(jj + 1) * 128], vec[:, gm_i, j:j + 1], vec[:, sh_i, j:j + 1],
                       ALU.mult, ALU.add, [ptk, "vec%d" % gm_i, "vec%d" % sh_i], [hk])
                    sub(P, 15 + jj)
            return xt, xk, hT, hk

        def proj(hT, hk, w, wk, c0, c1, ps, pk):
            for j in range(8):
                mm(ps[:, 0:c1 - c0], hT[:, j, :], w[:, j, c0:c1], j == 0, j == 7, [hk, wk], [pk])

        def kv_stage(hT, hk, wkv, R, rp, rpk, latent):
            Kall, Kk = R["Kall"].next()
            Vret, Vk = R["Vret"].next()
            Vp, Vpk = R["Vp"].next()
            G, Gk = R["G"].next()
            pg, pgk = psS()
            proj(hT, hk, wkv, "wkv", 2048, 2064, pg, pgk)
            tt("dve", G[:, 0:16], pg[:, 0:16], gbias[:], ALU.add, [pgk, "gbias"], [Gk])
            act(G[:, 16:24], G[:, 8:16], AF.Exp, [Gk], [Gk], scale=-1.0)
            act(G[:, 24:32], G[:, 16:24], AF.Ln, [Gk], [Gk], bias=1.0)
            pc, pck = psS()
            mm(pc[:, 0:4], cst[:, C_TRIF:C_TRIF + 128], G[:, 24:28], True, True, ["cst", Gk], [pck])
            mm(pc[:, 4:8], cst[:, C_TRIB:C_TRIB + 128], G[:, 28:32], True, True, ["cst", Gk], [pck])
            mm(pc[:, 8:16], cst[:, C_ONES:C_ONES + 128], G[:, 24:32], True, True, ["cst", Gk], [pck])
            act(G[:, 32:48], pc[:, 0:16], AF.Exp, [pck], [Gk], scale=-1.0)
            tt("dve", G[:, 48:56], G[:, 0:8], pc[:, 0:8], ALU.add, [Gk, pck], [Gk])
            act(G[:, 56:64], G[:, 48:56], AF.Exp, [Gk], [Gk])
            pa, pk = psA.next()
            proj(hT, hk, wkv, "wkv", 0, 512, pa, pk)
            if latent:
                rope(pa, pk, Kall[:, 0:4, :], Kk, rp, rpk, KS, R)
            else:
                act(Kall[:, 0:4, :].rearrange("p h d -> p (h d)"), pa[:, :], AF.Copy, [pk], [Kk], scale=KS)
            pa, pk = psA.next()
            proj(hT, hk, wkv, "wkv", 512, 1024, pa, pk)
            cp("dve", Vret[:].rearrange("p h d -> p (h d)"), pa[:, :], [pk], [Vk])
            pa, pk = psA.next()
            proj(hT, hk, wkv, "wkv", 1024, 1536, pa, pk)
            act(Kall[:, 4:8, :].rearrange("p h d -> p (h d)"), pa[:, :], AF.Copy, [pk], [Kk], scale=KS)
            pa, pk = psA.next()
            proj(hT, hk, wkv, "wkv", 1536, 2048, pa, pk)
            pav = pa[:, :].rearrange("p (h d) -> p h d", h=4)
            for di in range(2):
                a_ap = G[:, 56 + 4 * di:60 + 4 * di]
                tt("dve", Vp[:, di, :, 0:128], pav, a_ap.unsqueeze(2).broadcast_to([128, 4, 128]), ALU.mult,
                   [pk, Gk], [Vpk])
                cp("pool", Vp[:, di, :, 128:129], a_ap.unsqueeze(2), [Gk], [Vpk])
            return Kall, Kk, Vret, Vk, Vp, Vpk, G, Gk

        def rope(pa, pk, out3, outk, rp, rpk, scale, R):
            A, Ak = R["ropeA"].next()
            B, Bk = R["ropeB"].next()
            pv = pa[:, :].rearrange("p (h d) -> p h d", h=4)
            cos_b = rp[:, 0:128].unsqueeze(1).broadcast_to([128, 4, 128])
            stt("dve", A[:], pv, scale, cos_b, ALU.mult, ALU.mult, [pk, rpk], [Ak])
            stt("dve", B[:, :, 0:64], pv[:, :, 64:128], scale,
                rp[:, 128:192].unsqueeze(1).broadcast_to([128, 4, 64]), ALU.mult, ALU.mult, [pk, rpk], [Bk])
            stt("dve", B[:, :, 64:128], pv[:, :, 0:64], scale,
                rp[:, 192:256].unsqueeze(1).broadcast_to([128, 4, 64]), ALU.mult, ALU.mult, [pk, rpk], [Bk])
            tt("pool", out3, A[:], B[:], ALU.add, [Ak, Bk], [outk])

        def state_update(di, Kall, Kk, Vret, Vk, Vp, Vpk, G, Gk, S, Sk, Sbf, Sbfk, R):
            Kw, Kwk = R["Kw"].next()
            toend = rconst[:, 4 * di:4 * di + 4]
            dd = G[:, 40 + 4 * di:44 + 4 * di]
            tt("pool", Kw[:, 0:4, :], Kall[:, 0:4, :], toend.unsqueeze(2).broadcast_to([128, 4, 128]), ALU.mult,
               [Kk, "rconst"], [Kwk])
            tt("pool", Kw[:, 4:8, :], Kall[:, 4:8, :], dd.unsqueeze(2).broadcast_to([128, 4, 128]), ALU.mult,
               [Kk, Gk], [Kwk])
            for hi in range(8):
                ps, psk = psS()
                if hi < 4:
                    W = 128
                    rhs, rk = Vret[:, hi, :], Vk
                    dec, dk = rconst[:, 8 + 4 * di + hi:9 + 4 * di + hi], "rconst"
                else:
                    W = 129
                    rhs, rk = Vp[:, di, hi - 4, 0:129], Vpk
                    dec, dk = G[:, 40 + 4 * di + hi - 4:41 + 4 * di + hi - 4], Gk
                mm(ps[:, 0:W], Kw[:, hi, :], rhs, True, True, [Kwk, rk], [psk])
                stt("dve", S[:, hi, 0:W], S[:, hi, 0:W], dec, ps[:, 0:W], ALU.mult, ALU.add,
                    [(Sk, hi), dk, psk], [(Sk, hi)])
                cp("act", Sbf[:, hi, 0:W], S[:, hi, 0:W], [(Sk, hi)], [(Sbfk, hi)])

        with ExitStack() as kvscope:
            wkv = sb("wkv", [128, 8, 2064], BF16, scope=kvscope)
            with ExitStack() as ph:
                load_cast(wkv, wkv_h, 8, 2064, "wkv", scope=ph)
                P.emit(); chk(1)

            with ExitStack() as ph:
                R = {
                    "x": Rot(ph, nc.sbuf_tensor, "x", 2, [128, D], F32),
                    "ss": Rot(ph, nc.sbuf_tensor, "ss", 2, [128, 4], F32),
                    "xn": Rot(ph, nc.sbuf_tensor, "xn", 2, [128, D], BF16),
                    "hT": Rot(ph, nc.sbuf_tensor, "hT", 2, [128, 8, 128], BF16),
                    "Kall": Rot(ph, nc.sbuf_tensor, "Kall", 2, [128, 8, 128], BF16),
                    "Vret": Rot(ph, nc.sbuf_tensor, "Vret", 2, [128, 4, 128], BF16),
                    "Vp": Rot(ph, nc.sbuf_tensor, "Vp", 2, [128, 2, 4, 130], BF16),
                    "G": Rot(ph, nc.sbuf_tensor, "G", 2, [128, 64], F32),
                    "Kw": Rot(ph, nc.sbuf_tensor, "Kw", 2, [128, 8, 128], BF16),
                    "ropeA": Rot(ph, nc.sbuf_tensor, "ropeA", 2, [128, 4, 128], F32),
                    "ropeB": Rot(ph, nc.sbuf_tensor, "ropeB", 2, [128, 4, 128], F32),
                    "rp": Rot(ph, nc.sbuf_tensor, "rp", 2, [128, 256], F32),
                }
                for di, order in ((0, (0, 1)), (1, (1, 0))):
                    for ci in order:
                        xt, xk, hT, hk = front_end(ctx_h[ci * 128:(ci + 1) * 128, :], 2, 3, R, "x")
                        sub(P, 0)
                        Kall, Kk, Vret, Vk, Vp, Vpk, G, Gk = kv_stage(hT, hk, wkv, R, None, None, False)
                        sub(P, 1)
                        if di == 0:
                            state_update(0, Kall, Kk, Vret, Vk, Vp, Vpk, G, Gk, S_f, "S_f", Sf_bf, "Sf_bf", R)
                            sub(P, 2)
                        else:
                            state_update(1, Kall, Kk, Vret, Vk, Vp, Vpk, G, Gk, S_b, "S_b", Sb_bf[0], "Sb_bf0", R)
                cur = 0
                for i in range(NCH - 1, -1, -1):
                    P.add("sp", lambda e, i=i, cur=cur: e.dma_start(
                        out=sb_h[i, :, :], in_=Sb_bf[cur][:].rearrange("p h v -> p (h v)")),
                        [("Sb_bf%d" % cur, h) for h in range(8)], [("sbh", i)], stream="sbst%d" % cur)
                    if i == 0:
                        break
                    rp, rpk = R["rp"].next()
                    dma(rp[:], rope_h[i, :, :], [], [rpk], "rp%d" % rpk[1])
                    xt, xk, hT, hk = front_end(x_h[i * 128:(i + 1) * 128, :], 0, 1, R, "x")
                    Kall, Kk, Vret, Vk, Vp, Vpk, G, Gk = kv_stage(hT, hk, wkv, R, rp, rpk, True)
                    nxt = 1 - cur
                    state_update(1, Kall, Kk, Vret, Vk, Vp, Vpk, G, Gk, S_b, "S_b", Sb_bf[nxt], "Sb_bf%d" % nxt, R)
                    cur = nxt
                    if i % 4 == 0:
                        P.emit()
                P.emit(); chk(2)

            with ExitStack() as ph:
                wq = sb("wq", [128, 8, 1024], BF16, scope=ph)
                with ExitStack() as ph2:
                    load_cast(wq, wq_h, 8, 1024, "wq", scope=ph2)
                    P.emit(); chk(3)
                R = {
                    "x": Rot(ph, nc.sbuf_tensor, "x", 2, [128, D], F32),
                    "ss": Rot(ph, nc.sbuf_tensor, "ss", 2, [128, 4], F32),
                    "xn": Rot(ph, nc.sbuf_tensor, "xn", 2, [128, D], BF16),
                    "hT": Rot(ph, nc.sbuf_tensor, "hT", 2, [128, 8, 128], BF16),
                    "Kall": Rot(ph, nc.sbuf_tensor, "Kall", 2, [128, 8, 128], BF16),
                    "Qall": Rot(ph, nc.sbuf_tensor, "Qall", 2, [128, 8, 128], BF16),
                    "Vret": Rot(ph, nc.sbuf_tensor, "Vret", 2, [128, 4, 128], BF16),
                    "Vp": Rot(ph, nc.sbuf_tensor, "Vp", 2, [128, 2, 4, 130], BF16),
                    "G": Rot(ph, nc.sbuf_tensor, "G", 2, [128, 64], F32),
                    "Kw": Rot(ph, nc.sbuf_tensor, "Kw", 2, [128, 8, 128], BF16),
                    "ropeA": Rot(ph, nc.sbuf_tensor, "ropeA", 2, [128, 4, 128], F32),
                    "ropeB": Rot(ph, nc.sbuf_tensor, "ropeB", 2, [128, 4, 128], F32),
                    "rp": Rot(ph, nc.sbuf_tensor, "rp", 2, [128, 256], F32),
                    "kT": Rot(ph, nc.sbuf_tensor, "kT", 2, [128, 8, 128], BF16),
                    "qT": Rot(ph, nc.sbuf_tensor, "qT", 2, [128, 8, 128], BF16),
                    "qfb": Rot(ph, nc.sbuf_tensor, "qfb", 2, [128, 8, 128], BF16),
                    "Pm": Rot(ph, nc.sbuf_tensor, "Pm", 4, [128, 2, 128], BF16),
                    "sbl": Rot(ph, nc.sbuf_tensor, "sbl", 2, [128, 8, 130], BF16),
                    "raw": Rot(ph, nc.sbuf_tensor, "raw", 2, [128, D], F32),
                    "O": Rot(ph, nc.sbuf_tensor, "O", 2, [128, 8, 129], F32),
                    "dn": Rot(ph, nc.sbuf_tensor, "dn", 2, [128, 16], F32),
                    "H": Rot(ph, nc.sbuf_tensor, "H", 2, [128, 8, 128], F32),
                }
                for i in range(NCH):
                    sbl, sblk = R["sbl"].next()
                    dma(sbl[:].rearrange("p h v -> p (h v)"), sb_h[i, :, :], [("sbh", i)], [sblk],
                        "sbl%d" % sblk[1], q="pool")
                    rp, rpk = R["rp"].next()
                    dma(rp[:], rope_h[i, :, :], [], [rpk], "rp%d" % rpk[1])
                    xt, xk, hT, hk = front_end(x_h[i * 128:(i + 1) * 128, :], 0, 1, R, "x")
                    Kall, Kk, Vret, Vk, Vp, Vpk, G, Gk = kv_stage(hT, hk, wkv, R, rp, rpk, True)
                    Qall, Qk = R["Qall"].next()
                    pa, pk = psA.next()
                    proj(hT, hk, wq, "wq", 0, 512, pa, pk)
                    rope(pa, pk, Qall[:, 0:4, :], Qk, rp, rpk, 1.0, R)
                    pa, pk = psA.next()
                    proj(hT, hk, wq, "wq", 512, 1024, pa, pk)
                    cp("act", Qall[:, 4:8, :].rearrange("p h d -> p (h d)"), pa[:, :], [pk], [Qk])
                    kT, kTk = R["kT"].next()
                    qT, qTk = R["qT"].next()
                    for src, srck, dst, dstk, ev in ((Kall, Kk, kT, kTk, "dve"), (Qall, Qk, qT, qTk, "act")):
                        for half in range(2):
                            pt, ptk = psT()
                            for jj in range(4):
                                tr(pt[:, jj * 128:(jj + 1) * 128], src[:, half * 4 + jj, :], [srck], [ptk])
                            cp(ev, dst[:, half * 4:half * 4 + 4, :].rearrange("p h d -> p (h d)"), pt[:, :],
                               [ptk], [dstk])
                    qfb, qfbk = R["qfb"].next()
                    tt("pool", qfb[:, 0:4, :], qT[:, 0:4, :], cfb[:, 0:4, :], ALU.mult, [qTk, "cfb"], [qfbk])
                    tt("pool", qfb[:, 4:8, :], qT[:, 0:4, :], cfb[:, 4:8, :], ALU.mult, [qTk, "cfb"], [qfbk])
                    raw, rawk = R["raw"].next()
                    O, Ok = R["O"].next()
                    for h in range(4):
                        ps, psk = psS()
                        mm(ps[:, 0:128], kT[:, h, :], qT[:, h, :], True, True, [kTk, qTk], [psk])
                        Pm, Pmk = R["Pm"].next()
                        tt("dve", Pm[:, 0, :], ps[:, 0:128], Mret[:, h, :], ALU.mult, [psk, "Mret"], [Pmk])
                        po, pok = psS()
                        mm(po[:, 0:128], Pm[:, 0, :], Vret[:, h, :], True, False, [Pmk, Vk], [pok])
                        mm(po[:, 0:128], qfb[:, h, :], Sf_bf[:, h, 0:128], False, False, [qfbk, ("Sf_bf", h)], [pok])
                        mm(po[:, 0:128], qfb[:, 4 + h, :], sbl[:, h, 0:128], False, True, [qfbk, sblk], [pok])
                        cp("act", raw[:, h * 128:(h + 1) * 128], po[:, 0:128], [pok], [rawk])
                    for h in range(4):
                        ps, psk = psS()
                        mm(ps[:, 0:128], kT[:, 4 + h, :], qT[:, 4 + h, :], True, True, [kTk, qTk], [psk])
                        Pm, Pmk = R["Pm"].next()
                        tt("dve", Pm[:, 0, :], ps[:, 0:128], cst[:, C_TRIF:C_TRIF + 128], ALU.mult, [psk, "cst"], [Pmk])
                        tt("dve", Pm[:, 1, :], ps[:, 0:128], cst[:, C_TRIB:C_TRIB + 128], ALU.mult, [psk, "cst"], [Pmk])
                        for di in range(2):
                            po, pok = psS()
                            st_ap = Sf_bf[:, 4 + h, 0:129] if di == 0 else sbl[:, 4 + h, 0:129]
                            st_k = ("Sf_bf", 4 + h) if di == 0 else sblk
                            mm(po[:, 0:129], Pm[:, di, :], Vp[:, di, h, 0:129], True, False, [Pmk, Vpk], [pok])
                            mm(po[:, 0:129], qT[:, 4 + h, :], st_ap, False, True, [qTk, st_k], [pok])
                            ts("dve", O[:, di * 4 + h, :], po[:, 0:129], G[:, 32 + 4 * di + h:33 + 4 * di + h], None,
                               ALU.mult, None, [pok, Gk], [Ok])
                    dn, dnk = R["dn"].next()
                    act(dn[:, 0:8], O[:, :, 128], AF.Abs, [Ok], [dnk])
                    ts("dve", dn[:, 0:8], dn[:, 0:8], 1.0, None, ALU.max, None, [dnk], [dnk])
                    P.add("dve", lambda e, dn=dn: e.reciprocal(out=dn[:, 8:16], in_=dn[:, 0:8]), [dnk], [dnk])
                    H, Hk = R["H"].next()
                    tt("dve", H[:], O[:, :, 0:128], dn[:, 8:16].unsqueeze(2).broadcast_to([128, 8, 128]), ALU.mult,
                       [Ok, dnk], [Hk])
                    tt("pool", raw[:, 512:1024].rearrange("p (h d) -> p h d", h=4), H[:, 0:4, :], H[:, 4:8, :],
                       ALU.add, [Hk], [rawk])
                    dma(raw_h[i * 128:(i + 1) * 128, :], raw[:], [rawk], [("rawh", i)], "raw%d" % rawk[1])
                    state_update(0, Kall, Kk, Vret, Vk, Vp, Vpk, G, Gk, S_f, "S_f", Sf_bf, "Sf_bf", R)
                    if i % 4 == 3:
                        P.emit()
                P.emit(); chk(4)

        kvs.close()
        with ExitStack() as ph:
            wpost = sb("wpost", [128, 8, 3072], BF16, scope=ph)
            wrup = sb("wrup", [128, 4, D], BF16, scope=ph)
            wmup = sb("wmup", [128, 4, D], BF16, scope=ph)
            wout = sb("wout", [128, 8, D], BF16, scope=ph)
            with ExitStack() as ph2:
                st = Rot(ph2, nc.sbuf_tensor, "wstF2", 2, [128, 3072], F32)
                load_cast(wpost, wpost_h, 8, 3072, "wpost", st=st)
                load_cast(wrup, wrup_h, 4, D, "wrup", st=st)
                load_cast(wmup, wmup_h, 4, D, "wmup", st=st)
                load_cast(wout, wout_h, 8, D, "wout", colscale=bc[:, 0, :], st=st)
                P.emit(); chk(5)
            R = {
                "x": Rot(ph, nc.sbuf_tensor, "x", 2, [128, D], F32),
                "ss": Rot(ph, nc.sbuf_tensor, "ss", 2, [128, 4], F32),
                "xn": Rot(ph, nc.sbuf_tensor, "xn", 2, [128, D], BF16),
                "hT": Rot(ph, nc.sbuf_tensor, "hT", 2, [128, 8, 128], BF16),
                "raw": Rot(ph, nc.sbuf_tensor, "raw", 2, [128, D], F32),
                "sig": Rot(ph, nc.sbuf_tensor, "sig", 1, [128, 3072], F32),
                "e1": Rot(ph, nc.sbuf_tensor, "e1", 2, [128, 512], F32),
                "e2": Rot(ph, nc.sbuf_tensor, "e2", 2, [128, 512], F32),
                "hn": Rot(ph, nc.sbuf_tensor, "hn", 2, [128, 32], F32),
                "t": Rot(ph, nc.sbuf_tensor, "t", 1, [128, D], F32),
                "y": Rot(ph, nc.sbuf_tensor, "y", 2, [128, D], BF16),
                "yT": Rot(ph, nc.sbuf_tensor, "yT", 2, [128, 8, 128], BF16),
                "m1": Rot(ph, nc.sbuf_tensor, "m1", 1, [128, D], F32),
                "z": Rot(ph, nc.sbuf_tensor, "z", 2, [128, 512], F32),
                "m2": Rot(ph, nc.sbuf_tensor, "m2", 1, [128, D], F32),
                "mg": Rot(ph, nc.sbuf_tensor, "mg", 2, [128, D], BF16),
                "mT": Rot(ph, nc.sbuf_tensor, "mT", 2, [128, 8, 128], BF16),
                "x1": Rot(ph, nc.sbuf_tensor, "x1", 2, [128, D], F32),
            }
            for i in range(NCH):
                raw, rawk = R["raw"].next()
                dma(raw[:], raw_h[i * 128:(i + 1) * 128, :], [("rawh", i)], [rawk], "rawl%d" % rawk[1], q="pool")
                xt, xk, hT, hk = front_end(x_h[i * 128:(i + 1) * 128, :], 0, 1, R, "x")
                sig, sigk = R["sig"].next()
                t, tk = R["t"].next()
                for blk in range(6):
                    pa, pk = psA.next()
                    proj(hT, hk, wpost, "wpost", blk * 512, (blk + 1) * 512, pa, pk)
                    e1, e1k = R["e1"].next()
                    e2, e2k = R["e2"].next()
                    act(e1[:], pa[:, :], AF.Exp, [pk], [e1k], scale=-1.0)
                    act(e2[:], e1[:], AF.Ln, [e1k], [e2k], bias=1.0)
                    act(sig[:, blk * 512:(blk + 1) * 512], e2[:], AF.Exp, [e2k], [(sigk, blk)], scale=-1.0)
                    if blk == 0:
                        tt("dve", t[:, 0:512], pa[:, :], sig[:, 0:512], ALU.mult, [pk, (sigk, 0)], [(tk, 0)])
                hn, hnk = R["hn"].next()
                y, yk = R["y"].next()
                for h in range(4):
                    act(junk[:, 0:128], raw[:, h * 128:(h + 1) * 128], AF.Square, [rawk], ["junk", hnk],
                        scale=KS, accum=hn[:, h:h + 1])
                rsqrt_act(hn[:, 8:12], hn[:, 0:4], [hnk], [hnk], hn[:, 4:8], hnk)
                rawv = raw[:, 0:512].rearrange("p (h d) -> p h d", h=4)
                tt("dve", t[:, 512:1024].rearrange("p (h d) -> p h d", h=4), rawv,
                   hn[:, 8:12].unsqueeze(2).broadcast_to([128, 4, 128]), ALU.mult, [rawk, hnk], [(tk, 1)])
                tt("pool", y[:, 0:512], t[:, 512:1024], t[:, 0:512], ALU.mult, [(tk, 0), (tk, 1)], [(yk, 0)])
                m1, m1k = R["m1"].next()
                z, zk = R["z"].next()
                tt("pool", z[:, 0:512], raw[:, 512:1024], sig[:, 512:1024], ALU.mult, [rawk, (sigk, 1)], [zk])
                for h in range(4):
                    act(junk[:, 0:128], z[:, h * 128:(h + 1) * 128], AF.Square, [zk], ["junk", hnk],
                        scale=KS, accum=hn[:, 16 + h:17 + h])
                rsqrt_act(hn[:, 24:28], hn[:, 16:20], [hnk], [hnk], hn[:, 20:24], hnk)
                tt("dve", y[:, 512:1024].rearrange("p (h d) -> p h d", h=4),
                   z[:, 0:512].rearrange("p (h d) -> p h d", h=4),
                   hn[:, 24:28].unsqueeze(2).broadcast_to([128, 4, 128]), ALU.mult, [zk, hnk], [(yk, 1)])
                yT, yTk = R["yT"].next()
                for half in range(2):
                    pt, ptk = psT()
                    for jj in range(4):
                        j = half * 4 + jj
                        tr(pt[:, jj * 128:(jj + 1) * 128], y[:, j * 128:(j + 1) * 128], [(yk, half)], [ptk])
                    cp("act" if half == 0 else "dve", yT[:, half * 4:half * 4 + 4, :].rearrange("p h d -> p (h d)"),
                       pt[:, :], [ptk], [(yTk, half)])
                m2, m2k = R["m2"].next()
                mgt, mgk = R["mg"].next()
                for cb in range(2):
                    pa, pk = psA.next()
                    for j in range(4):
                        mm(pa[:, :], yT[:, j, :], wrup[:, j, cb * 512:(cb + 1) * 512], j == 0, j == 3,
                           [(yTk, 0), "wrup"], [pk])
                    tt("dve", m1[:, cb * 512:(cb + 1) * 512], pa[:, :], sig[:, 1024 + cb * 512:1536 + cb * 512],
                       ALU.mult, [pk, (sigk, 2 + cb)], [(m1k, cb)])
                    pa, pk = psA.next()
                    for j in range(4):
                        mm(pa[:, :], yT[:, 4 + j, :], wmup[:, j, cb * 512:(cb + 1) * 512], j == 0, j == 3,
                           [(yTk, 1), "wmup"], [pk])
                    tt("dve", m2[:, cb * 512:(cb + 1) * 512], pa[:, :], sig[:, 2048 + cb * 512:2560 + cb * 512],
                       ALU.mult, [pk, (sigk, 4 + cb)], [(m2k, cb)])
                    tt("pool", mgt[:, cb * 512:(cb + 1) * 512], m1[:, cb * 512:(cb + 1) * 512],
                       m2[:, cb * 512:(cb + 1) * 512], ALU.add, [(m1k, cb), (m2k, cb)], [(mgk, cb)])
                mT, mTk = R["mT"].next()
                for half in range(2):
                    pt, ptk = psT()
                    for jj in range(4):
                        j = half * 4 + jj
                        tr(pt[:, jj * 128:(jj + 1) * 128], mgt[:, j * 128:(j + 1) * 128], [(mgk, half)], [ptk])
                    cp("act" if half == 0 else "dve", mT[:, half * 4:half * 4 + 4, :].rearrange("p h d -> p (h d)"),
                       pt[:, :], [ptk], [(mTk, half)])
                x1, x1k = R["x1"].next()
                for cb in range(2):
                    pa, pk = psA.next()
                    for j in range(8):
                        mm(pa[:, :], mT[:, j, :], wout[:, j, cb * 512:(cb + 1) * 512], j == 0, j == 7,
                           [(mTk, j // 4), "wout"], [pk])
                    tt("dve", x1[:, cb * 512:(cb + 1) * 512], pa[:, :], xt[:, cb * 512:(cb + 1) * 512], ALU.add,
                       [pk, xk], [(x1k, cb)])
                dma(x1_h[i * 128:(i + 1) * 128, :], x1[:], [(x1k, 0), (x1k, 1)], [("x1h", i)], "x1s%d" % x1k[1])
                sst, ssk = R["ss"].next()
                act(junk[:], x1[:], AF.Square, [(x1k, 0), (x1k, 1)], ["junk", ssk], scale=1.0 / 32.0,
                    accum=sst[:, 0:1])
                rsqrt_act(rstd2[:, i:i + 1], sst[:, 0:1], [ssk], [("rstd2", i)], sst[:, 1:2], ssk)
                if i % 4 == 3:
                    P.emit()
            P.emit(); chk(6)

        with ExitStack() as ph:
            wffi = sb("wffi", [128, 8, 2 * HID], BF16, scope=ph)
            wffo = sb("wffo", [128, 22, D], BF16, scope=ph)
            with ExitStack() as ph2:
                st = Rot(ph2, nc.sbuf_tensor, "wstF3", 2, [128, 3072], F32)
                load_cast(wffi, wffi_h, 8, 2 * HID, "wffi", st=st)
                load_cast(wffo, wffo_h, 22, D, "wffo", colscale=bc[:, 1, :], st=st)
                P.emit(); chk(7)
            GC = 2 if NCH % 2 == 0 else 1
            GW = GC * 128
            R = {
                "x1": Rot(ph, nc.sbuf_tensor, "x1g", 2, [128, GC, D], F32),
                "xn": Rot(ph, nc.sbuf_tensor, "xn2", 1, [128, GC, D], BF16),
                "hT": Rot(ph, nc.sbuf_tensor, "h2T", 1, [128, 8, GW], BF16),
                "actT": Rot(ph, nc.sbuf_tensor, "actT", 1, [128, 22, GW], BF16),
                "e1": Rot(ph, nc.sbuf_tensor, "f1", 2, [128, GW], F32),
                "e2": Rot(ph, nc.sbuf_tensor, "f2", 2, [128, GW], F32),
                "e3": Rot(ph, nc.sbuf_tensor, "f3", 2, [128, GW], F32),
                "e4": Rot(ph, nc.sbuf_tensor, "f4", 2, [128, GW], F32),
                "x2": Rot(ph, nc.sbuf_tensor, "x2", 1, [128, D], F32),
                "ss": Rot(ph, nc.sbuf_tensor, "ss3", 2, [128, 4], F32),
                "o": Rot(ph, nc.sbuf_tensor, "o", 2, [128, D], F32),
            }
            for g in range(NCH // GC):
                x1g, x1k = R["x1"].next()
                xn, xnk = R["xn"].next()
                for c in range(GC):
                    i = g * GC + c
                    dma(x1g[:, c, :], x1_h[i * 128:(i + 1) * 128, :], [("x1h", i)], [(x1k, c)],
                        "x1l%d_%d" % (x1k[1], c), q=("sp" if c == 0 else "pool"))
                    ts("dve", xn[:, c, :], x1g[:, c, :], rstd2[:, i:i + 1], None, ALU.mult, None,
                       [(x1k, c), ("rstd2", i)], [(xnk, c)])
                hT, hk = R["hT"].next()
                for j in range(8):
                    if j % 2 == 0:
                        pt, ptk = psT()
                    for c in range(GC):
                        o0 = (j % 2) * GW + c * 128
                        tr(pt[:, o0:o0 + 128], xn[:, c, j * 128:(j + 1) * 128], [(xnk, c)], [ptk])
                    ts("dve", hT[:, j, :], pt[:, (j % 2) * GW:(j % 2) * GW + GW], vec[:, 4, j:j + 1], vec[:, 5, j:j + 1],
                       ALU.mult, ALU.add, [ptk, "vec4", "vec5"], [hk])
                aT, aTk = R["actT"].next()
                for jj in range(22):
                    pa, pk = psA.next()
                    for half in range(2):
                        c0 = half * HID + jj * 128
                        for j in range(8):
                            mm(pa[:, half * GW:half * GW + GW], wffi[:, j, c0:c0 + 128], hT[:, j, :], j == 0, j == 7,
                               ["wffi", hk], [pk])
                    e1, e1k = R["e1"].next()
                    e2, e2k = R["e2"].next()
                    e3, e3k = R["e3"].next()
                    e4, e4k = R["e4"].next()
                    act(e1[:], pa[:, 0:GW], AF.Exp, [pk], [e1k], scale=-1.0)
                    act(e2[:], e1[:], AF.Ln, [e1k], [e2k], bias=1.0)
                    act(e3[:], e2[:], AF.Exp, [e2k], [e3k], scale=-1.0)
                    tt("dve", e4[:], pa[:, 0:GW], e3[:], ALU.mult, [pk, e3k], [e4k])
                    tt("dve", aT[:, jj, :], pa[:, GW:2 * GW], e4[:], ALU.mult, [pk, e4k], [(aTk, jj)])
                for c in range(GC):
                    i = g * GC + c
                    x2, x2k = R["x2"].next()
                    for cb in range(2):
                        pa, pk = psA.next()
                        for jj in range(22):
                            mm(pa[:, :], aT[:, jj, c * 128:(c + 1) * 128], wffo[:, jj, cb * 512:(cb + 1) * 512],
                               jj == 0, jj == 21, [(aTk, jj), "wffo"], [pk])
                        tt("dve", x2[:, cb * 512:(cb + 1) * 512], pa[:, :], x1g[:, c, cb * 512:(cb + 1) * 512],
                           ALU.add, [pk, (x1k, c)], [(x2k, cb)])
                    sst, ssk = R["ss"].next()
                    act(junk[:], x2[:], AF.Square, [(x2k, 0), (x2k, 1)], ["junk", ssk], scale=1.0 / 32.0,
                        accum=sst[:, 0:1])
                    rsqrt_act(sst[:, 2:3], sst[:, 0:1], [ssk], [ssk], sst[:, 1:2], ssk)
                    o, ok = R["o"].next()
                    stt("dve", o[:], x2[:], sst[:, 2:3], bc[:, 2, :], ALU.mult, ALU.mult,
                        [(x2k, 0), (x2k, 1), ssk, "fgbc"], [ok])
                    dma(y_h[i * 128:(i + 1) * 128, :], o[:], [ok], [("yh", i)], "ys%d" % ok[1])
                if g % 2 == 1:
                    P.emit()
            P.emit(); chk(8)


def _consts():
    c = np.zeros((128, NCST), np.float32)
    s = np.arange(128, dtype=np.float32)[:, None]
    l = np.arange(128, dtype=np.float32)[None, :]
    c[:, C_TRIF:C_TRIF + 128] = (s <= l)
    c[:, C_TRIB:C_TRIB + 128] = (s >= l)
    c[:, C_ONES:C_ONES + 128] = 1.0
    c[:, C_RELF:C_RELF + 128] = np.maximum(l - s, 0.0)
    c[:, C_RELB:C_RELB + 128] = np.maximum(s - l, 0.0)
    c[:, C_LP1:C_LP1 + 128] = l + 1.0
    c[:, C_LM:C_LM + 128] = 128.0 - l
    c[:, C_COLS] = 127.0 - s[:, 0]
    c[:, C_COLS + 1] = s[:, 0]
    c[0, C_SEL:C_SEL + 128] = 1.0
    c[0, C_I2] = 1.0
    c[1, C_I2 + 1] = 1.0
    c[:, C_ID:C_ID + 128] = np.eye(128, dtype=np.float32)
    return c


def _rope_tables(n_tok):
    t = np.arange(n_tok)
    rows = (t // 64).astype(np.float32)
    cols = (t % 64).astype(np.float32)
    inv = (np.float32(10000.0) ** (-np.arange(32, dtype=np.float32) / np.float32(32))).astype(np.float32)
    ang = np.concatenate([rows[:, None] * inv, cols[:, None] * inv], axis=-1).astype(np.float32)
    cos, sin = np.cos(ang).astype(np.float32), np.sin(ang).astype(np.float32)
    tab = np.concatenate([cos, cos, -sin, sin], axis=-1)
    return np.ascontiguousarray(tab.reshape(n_tok // 128, 128, 256))


_NC_CACHE = {}


def kernel(x, c, ctx, c_ctx, w_ada, b_ada, norm1_gain, norm2_gain, w_in, mlstm_gate_bias,
           ret_decay_logit, w_ret_up, w_ml_up, w_out, w_ffn_in, w_ffn_out, final_gain):
    f = lambda a: np.ascontiguousarray(np.asarray(a, dtype=np.float32))
    x, c, ctx, c_ctx = f(x), f(c), f(ctx), f(c_ctx)
    B, NT, _ = x.shape
    NCH = NT // 128
    w_in0 = f(w_in)[0]
    rq, rk, rv, rg = (w_in0[:, i * 512:(i + 1) * 512] for i in range(4))
    mq, mk, mv, mo = (w_in0[:, 2048 + i * 512:2048 + (i + 1) * 512] for i in range(4))
    mgc = w_in0[:, 4096:4112].reshape(1024, 4, 4)[:, [0, 2, 1, 3], :].reshape(1024, 16)
    bgr, bgm = w_in0[:, 4112:5136], w_in0[:, 5136:6160]
    w_kv = np.ascontiguousarray(np.concatenate([rk, rv, mk, mv, mgc], axis=1))
    w_q = np.ascontiguousarray(np.concatenate([rq, mq], axis=1))
    w_post = np.ascontiguousarray(np.concatenate([rg, mo, bgr, bgm], axis=1))
    gb = f(mlstm_gate_bias)[0][[0, 2, 1, 3], :].reshape(1, 16)
    gains = np.ascontiguousarray(np.concatenate(
        [f(norm1_gain)[0].reshape(8, 128).T, f(norm2_gain)[0].reshape(8, 128).T], axis=1))
    common = {
        "w_ada": f(w_ada)[0], "b_ada": f(b_ada)[0].reshape(1, -1), "gains": gains,
        "final_gain": f(final_gain).reshape(1, -1), "w_kv": w_kv, "w_q": w_q, "w_post": w_post,
        "gate_bias": np.ascontiguousarray(gb), "ret_decay": f(ret_decay_logit)[0].reshape(1, 8),
        "w_ret_up": f(w_ret_up)[0], "w_ml_up": f(w_ml_up)[0], "w_out": f(w_out)[0],
        "w_ffn_in": f(w_ffn_in)[0], "w_ffn_out": f(w_ffn_out)[0],
        "cst": _consts(), "rope": _rope_tables(NT),
    }
    in_maps = []
    for b in range(B):
        cc = np.stack([c[b], c_ctx], axis=-1).reshape(8, 128, 2).transpose(1, 0, 2).reshape(128, 16)
        m = dict(common)
        m["x"] = x[b]
        m["ctx"] = ctx[b]
        m["cc"] = np.ascontiguousarray(cc)
        in_maps.append(m)
    if NCH not in _NC_CACHE:
        _NC_CACHE[NCH] = build_nc(NCH)
    res = run_bass_kernel_spmd(_NC_CACHE[NCH], in_maps, core_ids=list(range(B)))
    return np.stack([r["y"] for r in res.results], axis=0)
```
